# Optimizing a Trainium2 kernel written in Bass

```python
import math
import jax
import jax.numpy as jnp
from jax import lax
import numpy as np

D_MODEL = 1024
BATCH = 8
SEQ = 2048
DEPTH = 4

GRID_W = 64
CTX_LEN = 256
N_MIXERS = 3
S5_GROUP = 16
S5_GROUPS = D_MODEL // S5_GROUP
S5_STATE = 64
S5_DT_MIN = 1e-3
S5_DT_MAX = 1e-1
CONV_WIDTH = 31
CONV_PAD = CONV_WIDTH // 2
RWKV_HEAD = 64
RWKV_HEADS = D_MODEL // RWKV_HEAD
DECAY_LORA = max(32, int(round(1.8 * D_MODEL ** 0.5 / 32)) * 32)
ICLR_LORA = DECAY_LORA
GATE_LORA = max(32, int(round(0.6 * D_MODEL ** 0.8 / 32)) * 32)
FFN_HIDDEN = D_MODEL * 7 // 2
N_EXPERTS = 8
TOP_K = 2
RMS_EPS = 1e-6
LN_EPS = 1e-5
GN_EPS = 64e-5
KK_EPS = 1e-12
LAMBDA_RE_MAX = -1e-4

kernel_name = 'hybrid_s5_conformer_rwkv7_moe_dit'


def _rmsnorm(x, g):
    x32 = x.astype(jnp.float32)
    y = x32 * lax.rsqrt(jnp.mean(x32 * x32, axis=-1, keepdims=True) + RMS_EPS)
    return y.astype(x.dtype) * g


def _layernorm(x, g, b):
    x32 = x.astype(jnp.float32)
    mu = jnp.mean(x32, axis=-1, keepdims=True)
    xc = x32 - mu
    y = xc * lax.rsqrt(jnp.mean(xc * xc, axis=-1, keepdims=True) + LN_EPS)
    return y.astype(x.dtype) * g + b


def _modulate(h, shift, scale):
    return h * (1 + scale) + shift


def _flip_parts(t, lc):
    return jnp.concatenate([jnp.flip(t[:, :lc], axis=1), jnp.flip(t[:, lc:], axis=1)], axis=1)


def _restore_order(t, lc, start):
    return _flip_parts(t, lc) if start == 0 else jnp.flip(t, axis=1)


def _centred_shift(t):
    zero = jnp.zeros_like(t[:, :1])
    prev = jnp.concatenate([zero, t[:, :-1]], axis=1)
    nxt = jnp.concatenate([t[:, 1:], zero], axis=1)
    return 0.5 * (prev + nxt)


def _complex_affine_combine(e1, e2):
    a1r, a1i, b1r, b1i = e1
    a2r, a2i, b2r, b2i = e2
    return (a1r * a2r - a1i * a2i, a1r * a2i + a1i * a2r,
            a2r * b1r - a2i * b1i + b2r, a2r * b1i + a2i * b1r + b2i)


def _s5_states(ug, lam_re, lam_im, log_dt, b_re, b_im):
    lam_re = jnp.minimum(lam_re.astype(jnp.float32), LAMBDA_RE_MAX)
    lam_im = lam_im.astype(jnp.float32)
    dt = jnp.exp(log_dt.astype(jnp.float32))[:, None]
    mag = jnp.exp(lam_re * dt)
    abar_re = mag * jnp.cos(lam_im * dt)
    abar_im = mag * jnp.sin(lam_im * dt)
    inv_den = 1.0 / (lam_re * lam_re + lam_im * lam_im)
    gam_re = ((abar_re - 1.0) * lam_re + abar_im * lam_im) * inv_den
    gam_im = (abar_im * lam_re - (abar_re - 1.0) * lam_im) * inv_den
    b_re = b_re.astype(jnp.float32)
    b_im = b_im.astype(jnp.float32)
    bb_re = gam_re[..., None] * b_re - gam_im[..., None] * b_im
    bb_im = gam_re[..., None] * b_im + gam_im[..., None] * b_re
    bu_re = jnp.einsum('btgp,gnp->btgn', ug, bb_re)
    bu_im = jnp.einsum('btgp,gnp->btgn', ug, bb_im)
    shape = (1, ug.shape[1]) + abar_re.shape
    elems = (jnp.broadcast_to(abar_re, shape), jnp.broadcast_to(abar_im, shape), bu_re, bu_im)
    _, _, h_re, h_im = lax.associative_scan(_complex_affine_combine, elems, axis=1)
    return h_re, h_im


def _s5_readout(h_re, h_im, c_re, c_im):
    return (jnp.einsum('btgn,gpn->btgp', h_re, c_re.astype(jnp.float32))
            - jnp.einsum('btgn,gpn->btgp', h_im, c_im.astype(jnp.float32)))


def _s5_mixer(h_ctx, h_lat, lam_re, lam_im, log_dt, b_re, b_im, c_re, c_im, d_skip, glu_w, glu_b, ctx_out):
    lc = h_ctx.shape[1]
    start = 0 if ctx_out else lc
    u = jnp.concatenate([h_ctx, h_lat], axis=1)
    bsz, t_all, d = u.shape
    ug = u.reshape(bsz, t_all, S5_GROUPS, S5_GROUP).astype(jnp.float32)
    h_re, h_im = _s5_states(ug, lam_re[0], lam_im[0], log_dt[0], b_re[0], b_im[0])
    y = _s5_readout(h_re[:, start:], h_im[:, start:], c_re[0], c_im[0])
    h_re, h_im = _s5_states(_flip_parts(ug, lc), lam_re[1], lam_im[1], log_dt[1], b_re[1], b_im[1])
    y = y + _restore_order(_s5_readout(h_re[:, start:], h_im[:, start:], c_re[1], c_im[1]), lc, start)
    y = y + d_skip.astype(jnp.float32).reshape(S5_GROUPS, S5_GROUP) * ug[:, start:]
    z = jax.nn.gelu(y.reshape(bsz, t_all - start, d)).astype(h_lat.dtype)
    zz = z @ glu_w + glu_b
    out = zz[..., :d] * jax.nn.sigmoid(zz[..., d:])
    return (out[:, :lc] if ctx_out else None), out[:, lc - start:]


def _conv_module(h, pw1_w, pw1_b, dw_w, dw_b, ln_g, ln_b, pw2_w, pw2_b):
    d = h.shape[-1]
    u = h @ pw1_w + pw1_b
    u = u[..., :d] * jax.nn.sigmoid(u[..., d:])
    u = lax.conv_general_dilated(u, dw_w[:, None, :], window_strides=(1,),
                                 padding=((CONV_PAD, CONV_PAD),),
                                 dimension_numbers=('NWC', 'WIO', 'NWC'),
                                 feature_group_count=d) + dw_b
    u = jax.nn.silu(_layernorm(u, ln_g, ln_b))
    return u @ pw2_w + pw2_b


def _conformer_mixer(h_ctx, h_lat, rows, pw1_w, pw1_b, dw_w, dw_b, ln_g, ln_b, pw2_w, pw2_b, ctx_out):
    bsz, seq, d = h_lat.shape
    grid = h_lat.reshape(bsz * rows, GRID_W, d)
    y_lat = _conv_module(grid, pw1_w, pw1_b, dw_w, dw_b, ln_g, ln_b, pw2_w, pw2_b).reshape(bsz, seq, d)
    y_ctx = _conv_module(h_ctx, pw1_w, pw1_b, dw_w, dw_b, ln_g, ln_b, pw2_w, pw2_b) if ctx_out else None
    return y_ctx, y_lat


def _wkv7_scan(r, w, k, v, a, b):
    bsz, _, nh, n = r.shape

    def step(s, inp):
        r_t, w_t, k_t, v_t, a_t, b_t = inp
        sa = jnp.einsum('bhij,bhj->bhi', s, a_t)
        s = s * w_t[:, :, None, :] + sa[..., None] * b_t[:, :, None, :] + v_t[..., None] * k_t[:, :, None, :]
        return s, jnp.einsum('bhij,bhj->bhi', s, r_t)

    xs = tuple(jnp.moveaxis(z.astype(jnp.float32), 1, 0) for z in (r, w, k, v, a, b))
    s0 = jnp.zeros((bsz, nh, n, n), jnp.float32)
    _, y = lax.scan(step, s0, xs)
    return jnp.moveaxis(y, 0, 1)


def _rwkv7_mixer(h_ctx, h_lat, mu, w_r, w_k, w_v, w_o, w0, w1, w2, a0, a1, a2, g1, g2,
                 k_k, k_a, r_k, lnx_g, lnx_b, ctx_out):
    lc = h_ctx.shape[1]
    start = 0 if ctx_out else lc
    hcat = jnp.concatenate([h_ctx, h_lat], axis=1)
    bsz, t_all, d = hcat.shape
    t_out = t_all - start
    xx = jnp.concatenate([_centred_shift(h_ctx), _centred_shift(h_lat)], axis=1) - hcat
    xr, xw, xk, xv, xa, xg = (hcat + xx * mu[j] for j in range(6))

    def heads(z):
        return z.reshape(z.shape[0], z.shape[1], RWKV_HEADS, RWKV_HEAD)

    r = heads(xr @ w_r)
    k = heads(xk @ w_k)
    v = heads(xv @ w_v)
    kk = (k * k_k.reshape(RWKV_HEADS, RWKV_HEAD)).astype(jnp.float32)
    kk = kk * lax.rsqrt(jnp.sum(kk * kk, axis=-1, keepdims=True) + KK_EPS)
    k_a_h = k_a.reshape(RWKV_HEADS, RWKV_HEAD)
    ys = []
    keys = []
    for direction in range(2):
        log_w = -jax.nn.softplus(-(w0[direction] + jnp.tanh(xw @ w1[direction]) @ w2[direction])) - 0.5
        decay = heads(jnp.exp(-jnp.exp(log_w.astype(jnp.float32))))
        a = heads(jax.nn.sigmoid(a0[direction] + (xa @ a1[direction]) @ a2[direction]))
        k_dir = k * (1 + (a - 1) * k_a_h)
        seq = (r, decay, k_dir, v, -kk, kk * a)
        if direction == 1:
            seq = tuple(_flip_parts(z, lc) for z in seq)
        y_dir = _wkv7_scan(*seq)[:, start:]
        ys.append(_restore_order(y_dir, lc, start) if direction == 1 else y_dir)
        keys.append(k_dir[:, start:])
    y = ys[0] + ys[1]
    mean = jnp.mean(y, axis=-1, keepdims=True)
    yc = y - mean
    y = (yc * lax.rsqrt(jnp.mean(yc * yc, axis=-1, keepdims=True) + GN_EPS)).reshape(bsz, t_out, d)
    y = y.astype(hcat.dtype) * lnx_g + lnx_b
    k_mean = 0.5 * (keys[0] + keys[1])
    bonus = jnp.sum(r[:, start:] * k_mean * r_k, axis=-1, keepdims=True) * v[:, start:]
    g = jax.nn.sigmoid(xg[:, start:] @ g1) @ g2
    out = ((y + bonus.reshape(bsz, t_out, d)) * g) @ w_o
    return (out[:, :lc] if ctx_out else None), out[:, lc - start:]


def _swiglu(h, w1, w3, w2):
    return (jax.nn.silu(h @ w1) * (h @ w3)) @ w2


def _moe_swiglu(h, router, w1, w3, w2):
    logits = jnp.einsum('...d,de->...e', h, router).astype(jnp.float32)
    top_logits, top_idx = lax.top_k(logits, TOP_K)
    top_p = jax.nn.softmax(top_logits, axis=-1)
    gates = jnp.einsum('...k,...ke->...e', top_p,
                       jax.nn.one_hot(top_idx, N_EXPERTS, dtype=jnp.float32)).astype(h.dtype)
    out = gates[..., 0:1] * _swiglu(h, w1[0], w3[0], w2[0])
    for e in range(1, N_EXPERTS):
        out = out + gates[..., e:e + 1] * _swiglu(h, w1[e], w3[e], w2[e])
    return out


def setup_inputs(seed: int = 0) -> dict:
    key = jax.random.key(seed)
    ks = iter(jax.random.split(key, 64))
    f32 = jnp.float32

    def nrm(shape, scale):
        return scale * jax.random.normal(next(ks), shape, f32)

    def unif(shape, lo, hi):
        return jax.random.uniform(next(ks), shape, f32, lo, hi)

    d, f, e = D_MODEL, FFN_HIDDEN, N_EXPERTS
    g, p, n = S5_GROUPS, S5_GROUP, S5_STATE
    n_a = (DEPTH + 2) // 3
    n_b = (DEPTH + 1) // 3
    n_c = DEPTH // 3
    n_dense = (DEPTH + 1) // 2
    n_moe = DEPTH // 2
    decay_speed = -7.0 + 5.0 * (jnp.arange(d, dtype=f32) / (d - 1)) ** 0.85
    return {
        'x': nrm((BATCH, SEQ, d), 1.0),
        'c': nrm((BATCH, d), 1.0),
        'ctx': nrm((BATCH, CTX_LEN, d), 1.0),
        'c_ctx': nrm((d,), 1.0),
        'ada_w': nrm((DEPTH, d, 6 * d), 0.5 * d ** -0.5),
        'ada_b': nrm((DEPTH, 6 * d), 0.02),
        'norm_mix_g': 1.0 + nrm((DEPTH, d), 0.02),
        'norm_ffn_g': 1.0 + nrm((DEPTH, d), 0.02),
        'final_g': 1.0 + nrm((d,), 0.02),
        's5_lambda_re': -0.5 * (1.0 + nrm((n_a, 2, g, n), 0.02)),
        's5_lambda_im': math.pi * jnp.arange(n, dtype=f32) + nrm((n_a, 2, g, n), 0.02),
        's5_log_dt': unif((n_a, 2, g), math.log(S5_DT_MIN), math.log(S5_DT_MAX)),
        's5_b_re': nrm((n_a, 2, g, n, p), (2 * p) ** -0.5),
        's5_b_im': nrm((n_a, 2, g, n, p), (2 * p) ** -0.5),
        's5_c_re': nrm((n_a, 2, g, p, n), n ** -0.5),
        's5_c_im': nrm((n_a, 2, g, p, n), n ** -0.5),
        's5_d': nrm((n_a, d), 1.0),
        's5_glu_w': nrm((n_a, d, 2 * d), d ** -0.5),
        's5_glu_b': nrm((n_a, 2 * d), 0.02),
        'cv_pw1_w': nrm((n_b, d, 2 * d), d ** -0.5),
        'cv_pw1_b': nrm((n_b, 2 * d), 0.02),
        'cv_dw_w': nrm((n_b, CONV_WIDTH, d), CONV_WIDTH ** -0.5),
        'cv_dw_b': nrm((n_b, d), 0.02),
        'cv_ln_g': 1.0 + nrm((n_b, d), 0.02),
        'cv_ln_b': nrm((n_b, d), 0.02),
        'cv_pw2_w': nrm((n_b, d, d), d ** -0.5),
        'cv_pw2_b': nrm((n_b, d), 0.02),
        'rw_mu': unif((n_c, 6, d), 0.0, 1.0),
        'rw_w_r': nrm((n_c, d, d), d ** -0.5),
        'rw_w_k': nrm((n_c, d, d), d ** -0.5),
        'rw_w_v': nrm((n_c, d, d), d ** -0.5),
        'rw_w_o': nrm((n_c, d, d), d ** -0.5),
        'rw_w0': decay_speed + 0.5 + nrm((n_c, 2, d), 0.1),
        'rw_w1': nrm((n_c, 2, d, DECAY_LORA), d ** -0.5),
        'rw_w2': nrm((n_c, 2, DECAY_LORA, d), 0.1 * DECAY_LORA ** -0.5),
        'rw_a0': nrm((n_c, 2, d), 0.1),
        'rw_a1': nrm((n_c, 2, d, ICLR_LORA), d ** -0.5),
        'rw_a2': nrm((n_c, 2, ICLR_LORA, d), 0.1 * ICLR_LORA ** -0.5),
        'rw_g1': nrm((n_c, d, GATE_LORA), d ** -0.5),
        'rw_g2': nrm((n_c, GATE_LORA, d), GATE_LORA ** -0.5),
        'rw_k_k': 0.85 + nrm((n_c, d), 0.02),
        'rw_k_a': 1.0 + nrm((n_c, d), 0.02),
        'rw_r_k': nrm((n_c, RWKV_HEADS, RWKV_HEAD), 0.1),
        'rw_lnx_g': 1.0 + nrm((n_c, d), 0.02),
        'rw_lnx_b': nrm((n_c, d), 0.02),
        'ff_w1': nrm((n_dense, d, f), d ** -0.5),
        'ff_w3': nrm((n_dense, d, f), d ** -0.5),
        'ff_w2': nrm((n_dense, f, d), f ** -0.5),
        'moe_router': nrm((n_moe, d, e), d ** -0.5),
        'moe_w1': nrm((n_moe, e, d, f), d ** -0.5),
        'moe_w3': nrm((n_moe, e, d, f), d ** -0.5),
        'moe_w2': nrm((n_moe, e, f, d), f ** -0.5),
    }


def reference(x, c, ctx, c_ctx, ada_w, ada_b, norm_mix_g, norm_ffn_g, final_g,
              s5_lambda_re, s5_lambda_im, s5_log_dt, s5_b_re, s5_b_im, s5_c_re, s5_c_im,
              s5_d, s5_glu_w, s5_glu_b,
              cv_pw1_w, cv_pw1_b, cv_dw_w, cv_dw_b, cv_ln_g, cv_ln_b, cv_pw2_w, cv_pw2_b,
              rw_mu, rw_w_r, rw_w_k, rw_w_v, rw_w_o, rw_w0, rw_w1, rw_w2, rw_a0, rw_a1, rw_a2,
              rw_g1, rw_g2, rw_k_k, rw_k_a, rw_r_k, rw_lnx_g, rw_lnx_b,
              ff_w1, ff_w3, ff_w2, moe_router, moe_w1, moe_w3, moe_w2):
    rows = x.shape[1] // GRID_W
    silu_c = jax.nn.silu(c)
    silu_cc = jax.nn.silu(c_ctx)
    for i in range(DEPTH):
        last = i == DEPTH - 1
        m_lat = jnp.split((silu_c @ ada_w[i] + ada_b[i])[:, None, :], 6, axis=-1)
        m_ctx = jnp.split(silu_cc @ ada_w[i] + ada_b[i], 6, axis=-1)
        h_lat = _modulate(_rmsnorm(x, norm_mix_g[i]), m_lat[0], m_lat[1])
        h_ctx = _modulate(_rmsnorm(ctx, norm_mix_g[i]), m_ctx[0], m_ctx[1])
        j = i // N_MIXERS
        if i % N_MIXERS == 0:
            y_ctx, y_lat = _s5_mixer(h_ctx, h_lat, s5_lambda_re[j], s5_lambda_im[j], s5_log_dt[j],
                                     s5_b_re[j], s5_b_im[j], s5_c_re[j], s5_c_im[j], s5_d[j],
                                     s5_glu_w[j], s5_glu_b[j], not last)
        elif i % N_MIXERS == 1:
            y_ctx, y_lat = _conformer_mixer(h_ctx, h_lat, rows, cv_pw1_w[j], cv_pw1_b[j], cv_dw_w[j],
                                            cv_dw_b[j], cv_ln_g[j], cv_ln_b[j], cv_pw2_w[j], cv_pw2_b[j],
                                            not last)
        else:
            y_ctx, y_lat = _rwkv7_mixer(h_ctx, h_lat, rw_mu[j], rw_w_r[j], rw_w_k[j], rw_w_v[j], rw_w_o[j],
                                        rw_w0[j], rw_w1[j], rw_w2[j], rw_a0[j], rw_a1[j], rw_a2[j],
                                        rw_g1[j], rw_g2[j], rw_k_k[j], rw_k_a[j], rw_r_k[j],
                                        rw_lnx_g[j], rw_lnx_b[j], not last)
        x = x + m_lat[2] * y_lat
        if not last:
            ctx = ctx + m_ctx[2] * y_ctx
        fi = i // 2
        if i % 2 == 0:
            def ffn(h):
                return _swiglu(h, ff_w1[fi], ff_w3[fi], ff_w2[fi])
        else:
            def ffn(h):
                return _moe_swiglu(h, moe_router[fi], moe_w1[fi], moe_w3[fi], moe_w2[fi])
        x = x + m_lat[5] * ffn(_modulate(_rmsnorm(x, norm_ffn_g[i]), m_lat[3], m_lat[4]))
        if not last:
            ctx = ctx + m_ctx[5] * ffn(_modulate(_rmsnorm(ctx, norm_ffn_g[i]), m_ctx[3], m_ctx[4]))
    return _rmsnorm(x, final_g)
```

```python
import contextlib
import math
import numpy as np
import concourse.bass as bass
import concourse.mybir as mybir
from concourse.bass_utils import run_bass_kernel_spmd

F32 = mybir.dt.float32
BF16 = mybir.dt.bfloat16
I32 = mybir.dt.int32
ALU = mybir.AluOpType
AF = mybir.ActivationFunctionType

D = 1024
KC = 8
LC = 256
LL = 2048
T = LC + LL
F = 3584
FC = 28
NE = 8
TILES = [(0, 256, 1)] + [(256 + 512 * k, 512, 0) for k in range(4)]
TWO_PI = 2.0 * math.pi


def _box(ap):
    name = ap.tensor.name
    dims = list(ap.ap)
    off = ap.offset
    if str(ap.space).upper().find("DRAM") >= 0:
        lo = off
        hi = off
        for st, n in dims:
            if st >= 0:
                hi += st * (n - 1)
            else:
                lo += st * (n - 1)
        return (name, 0, 1, lo, hi + 1)
    pst, pn = dims[0]
    p0 = ap.start_partition()
    f_lo = off - p0 * pst
    f_hi = f_lo
    for st, n in dims[1:]:
        if st >= 0:
            f_hi += st * (n - 1)
        else:
            f_lo += st * (n - 1)
    return (name, p0, p0 + pn, f_lo, f_hi + 1)


def _ov(a, b):
    return a[1] < b[2] and b[1] < a[2] and a[3] < b[4] and b[3] < a[4]


def _cov(a, b):
    return a[1] <= b[1] and a[2] >= b[2] and a[3] <= b[3] and a[4] >= b[4]


class Prog:
    ENG = ('pe', 'dve', 'act', 'pool')

    def __init__(self, nc, es, n_dma_sems=16):
        self.nc = nc
        self.eng = {'pe': nc.tensor, 'dve': nc.vector, 'act': nc.scalar, 'pool': nc.gpsimd, 'sp': nc.sync}
        self.sem = {e: es.enter_context(nc.semaphore('c_' + e)) for e in self.ENG}
        self.cnt = {e: 0 for e in self.ENG}
        self.dq = {}
        for q in ('sp', 'act', 'pool'):
            self.dq[q] = [[es.enter_context(nc.semaphore('d_%s%d' % (q, i))), 0] for i in range(n_dma_sems)]
        self.dq_next = {q: 0 for q in self.dq}
        self.waited = {e: {} for e in ('pe', 'dve', 'act', 'pool', 'sp')}
        self.rec = {}
        self.n_inst = 0
        self.n_wait = 0

    def _deps(self, engine, reads, writes):
        deps = []
        for ap in reads:
            b = _box(ap)
            for (rb, tok, w) in self.rec.get(b[0], ()):
                if w and _ov(b, rb):
                    deps.append(tok)
        for ap in writes:
            b = _box(ap)
            for (rb, tok, w) in self.rec.get(b[0], ()):
                if _ov(b, rb):
                    if tok[0] == 'e' and tok[1] == engine:
                        continue
                    deps.append(tok)
        if engine == 'pe':
            deps = [t for t in deps if not (t[0] == 'e' and t[1] == 'pe')]
        return deps

    def _record(self, reads, writes, tok):
        for ap in writes:
            b = _box(ap)
            lst = self.rec.setdefault(b[0], [])
            lst[:] = [r for r in lst if not _cov(b, r[0])]
            lst.append((b, tok, True))
        for ap in reads:
            b = _box(ap)
            lst = self.rec.setdefault(b[0], [])
            lst[:] = [r for r in lst if not (not r[2] and tok[0] == 'e' and r[1][0] == 'e' and r[1][1] == tok[1] and _cov(b, r[0]))]
            lst.append((b, tok, False))

    def _wait(self, consumer, deps):
        ce = self.eng[consumer]
        need = {}
        for tok in deps:
            if tok[0] == 'e':
                key = ('e', tok[1])
                val = tok[2] + 1
            else:
                key = ('d', tok[1], tok[2])
                val = tok[3]
            if need.get(key, 0) < val:
                need[key] = val
        for key, val in need.items():
            if self.waited[consumer].get(key, 0) >= val:
                continue
            s = self.sem[key[1]] if key[0] == 'e' else self.dq[key[1]][key[2]][0]
            ce.wait_ge(s, val)
            self.n_wait += 1
            self.waited[consumer][key] = val

    def op(self, engine, fn, reads, writes):
        deps = self._deps(engine, reads, writes)
        self._wait(engine, deps)
        inst = fn(self.eng[engine])
        inst.then_inc(self.sem[engine], 1)
        idx = self.cnt[engine]
        self.cnt[engine] += 1
        self.n_inst += 1
        self._record(reads, writes, ('e', engine, idx))
        return inst

    def dma(self, q, out, in_, **kw):
        deps = self._deps(q, [in_], [out])
        k = self.dq_next[q]
        self.dq_next[q] = (k + 1) % len(self.dq[q])
        slot = self.dq[q][k]
        if slot[1] > 0:
            deps.append(('d', q, k, slot[1] * 16))
        self._wait(q, deps)
        self.eng[q].dma_start(out=out, in_=in_, **kw).then_inc(slot[0], 16)
        slot[1] += 1
        self.n_inst += 1
        self._record([in_], [out], ('d', q, k, slot[1] * 16))

    def barrier(self):
        deps = []
        for e in self.ENG:
            if self.cnt[e] > 0:
                deps.append(('e', e, self.cnt[e] - 1))
        for q in self.dq:
            for k, slot in enumerate(self.dq[q]):
                if slot[1] > 0:
                    deps.append(('d', q, k, slot[1] * 16))
        for c in ('pe', 'dve', 'act', 'pool', 'sp'):
            self._wait(c, deps)
        self.rec = {}

    def mm(self, out, lhsT, rhs, start, stop):
        return self.op('pe', lambda e: e.matmul(out, lhsT=lhsT, rhs=rhs, start=start, stop=stop),
                       [lhsT, rhs], [out])

    def act(self, out, in_, func, bias=None, scale=None, eng='act'):
        kw = {}
        rd = [in_]
        if bias is not None:
            kw['bias'] = bias
            if not isinstance(bias, (int, float)):
                rd.append(bias)
        if scale is not None:
            kw['scale'] = scale
            if not isinstance(scale, (int, float)):
                rd.append(scale)
        return self.op('act', lambda e: e.activation(out=out, in_=in_, func=func, **kw), rd, [out])

    def tt(self, eng, out, in0, in1, op):
        return self.op(eng, lambda e: e.tensor_tensor(out=out, in0=in0, in1=in1, op=op), [in0, in1], [out])

    def ts(self, eng, out, in0, s1, op0, s2=None, op1=None):
        rd = [in0]
        if not isinstance(s1, (int, float)):
            rd.append(s1)
        if s2 is not None and not isinstance(s2, (int, float)):
            rd.append(s2)
        if op1 is None:
            return self.op(eng, lambda e: e.tensor_scalar(out=out, in0=in0, scalar1=s1, scalar2=None, op0=op0), rd, [out])
        return self.op(eng, lambda e: e.tensor_scalar(out=out, in0=in0, scalar1=s1, scalar2=s2, op0=op0, op1=op1), rd, [out])

    def stt(self, out, in0, scalar, in1, op0, op1):
        rd = [in0, in1]
        if not isinstance(scalar, (int, float)):
            rd.append(scalar)
        return self.op('dve', lambda e: e.scalar_tensor_tensor(out=out, in0=in0, scalar=scalar, in1=in1, op0=op0, op1=op1),
                       rd, [out])

    def copy(self, eng, out, in_):
        if eng == 'act':
            return self.op('act', lambda e: e.activation(out=out, in_=in_, func=AF.Copy), [in_], [out])
        return self.op(eng, lambda e: e.tensor_copy(out=out, in_=in_), [in_], [out])

    def memset(self, eng, ap, val):
        return self.op(eng, lambda e: e.memset(ap, val), [], [ap])


TILES256 = [(256 * k, 256, 1 if k == 0 else 0) for k in range(9)]
PI_C = 3.1415925


class Builder:
    def __init__(self, n_layers=4, dbg=None, nb=1):
        self.nb = nb
        self.n_layers = n_layers
        self.dbg = dbg
        self.nc = bass.Bass("TRN2", target_bir_lowering=False)
        self.uid = 0

    def inp(self, name, shape, dt=F32):
        return self.nc.dram_tensor(name, list(shape), dt, kind="ExternalInput").ap()

    def scratch(self, name, shape, dt=F32):
        return self.nc.dram_tensor(name, list(shape), dt, kind="Internal").ap()

    def sb(self, name, shape, dt=F32, es=None):
        self.uid += 1
        return (es or self.es).enter_context(self.nc.sbuf_tensor("%s_%d" % (name, self.uid), list(shape), dt))

    @contextlib.contextmanager
    def scope(self):
        old = self.es
        with contextlib.ExitStack() as es:
            self.es = es
            yield es
            self.P.barrier()
        self.es = old

    def bank(self):
        b = self.ps[self.ps_i]
        self.ps_i = (self.ps_i + 1) % 8
        return b

    def build(self):
        nc = self.nc
        NL = self.n_layers
        xT_all = self.inp("xT", [self.nb, D, T])
        cvec_all = self.inp("cvec", [self.nb, 128, KC, 2])
        ada_w = self.inp("ada_w", [4, D, 6 * D])
        ada_b = self.inp("ada_b", [128, 4, 48])
        gmix = self.inp("gmix", [128, 4, KC])
        gffn = self.inp("gffn", [128, 4, KC])
        gfin = self.inp("gfin", [128, KC])
        self.s5_par = self.inp("s5_par", [2, 2, 128, 3, 32])
        self.s5_B = self.inp("s5_B", [2, 2, 32, 128, 2, 128])
        self.s5_C = self.inp("s5_C", [2, 2, 32, 128, 2, 128])
        self.s5_d = self.inp("s5_d", [128, 2, KC])
        self.s5_glu_w = self.inp("s5_glu_w", [2, D, 2 * D])
        self.s5_glu_b = self.inp("s5_glu_b", [128, 2, 16])
        self.cv_pw1_w = self.inp("cv_pw1_w", [1, D, 2 * D])
        self.cv_pw1_b = self.inp("cv_pw1_b", [128, 16])
        self.cv_dw_w = self.inp("cv_dw_w", [128, KC, 31])
        self.cv_vec = self.inp("cv_vec", [128, 4, KC])
        self.cv_pw2_w = self.inp("cv_pw2_w", [1, D, D])
        ff_w1 = self.inp("ff_w1", [2, D, F])
        ff_w3 = self.inp("ff_w3", [2, D, F])
        ff_w2 = self.inp("ff_w2", [2, F, D])
        self.moe_router = self.inp("moe_router", [2, 128, KC, NE])
        moe_w1 = self.inp("moe_w1", [2, NE, D, F])
        moe_w3 = self.inp("moe_w3", [2, NE, D, F])
        moe_w2 = self.inp("moe_w2", [2, NE, F, D])
        self.rwkv_decl()
        outT_all = nc.dram_tensor("outT", [self.nb, D, LL], F32, kind="ExternalOutput").ap()
        self.gb_scr = self.scratch("gb_scr", [NE, 128, T])
        self.v_scr = self.scratch("v_scr", [D, T])

        self.hb_scr = self.scratch("hb_scr", [128, KC, T], BF16)
        with contextlib.ExitStack() as es:
            self.es = es
            P = Prog(nc, es)
            self.P = P
            MOD = self.sb("MOD", [128, 4, 48, 2])
            ones = self.sb("ones", [128, 128])
            GM = self.sb("GM", [128, 4, KC])
            GF = self.sb("GF", [128, 4, KC])
            GFIN = self.sb("GFIN", [128, KC])
            AB = self.sb("AB", [128, 2, KC, 2])
            self.MOD, self.ones, self.AB = MOD, ones, AB
            self.ps = [es.enter_context(nc.psum_tensor("ps%d" % i, [128, 512], F32)) for i in range(8)]
            self.ps_i = 0
            P.memset('dve', ones[:], 1.0)
            P.dma('sp', GM[:], gmix)
            P.dma('sp', GF[:], gffn)
            P.dma('sp', GFIN[:], gfin)
            for bi in range(self.nb):
                xT = xT_all[bi]
                cvec = cvec_all[bi]
                outT = outT_all[bi]
                self.outT = outT
                self.adaln(ada_w, ada_b, cvec, NL)

                def ffn_layer(l, tiles):
                    self.norm_mod(l, 1, GF, want_router=(l % 2 == 1), fi=l // 2)
                    if l % 2 == 0:
                        self.ffn(l, [(ff_w1[l // 2], ff_w3[l // 2], ff_w2[l // 2])], tiles, moe=False)
                    else:
                        fi = l // 2
                        self.ffn(l, [(moe_w1[fi, e], moe_w3[fi, e], moe_w2[fi, e]) for e in range(NE)], tiles, moe=True)

                def emit_out():
                    if self.dbg is None and NL == 4:
                        self.final_norm(GFIN)
                    else:
                        for c in range(KC):
                            P.dma('sp', outT[c * 128:(c + 1) * 128, :], self.XA[:, c, LC:])

                with self.scope():
                    self.XA = self.sb("XA", [128, KC, T])
                    self.HB = self.sb("HB", [128, KC, T], BF16)
                    for c in range(KC):
                        P.dma('sp' if c % 2 == 0 else 'act', self.XA[:, c, :], xT[c * 128:(c + 1) * 128, :])
                    for l in range(min(NL, 2)):
                        self.norm_mod(l, 0, GM)
                        if l == 0:
                            self.s5_mixer(0, l, TILES)
                        else:
                            self.conv_mixer(l, TILES)
                        ffn_layer(l, TILES)
                    if NL > 2:
                        self.norm_mod(2, 0, GM)
                        P.dma('sp', self.xa_scr, self.XA[:])
                        P.dma('act', self.hb_scr, self.HB[:])
                    else:
                        emit_out()
                if NL > 2:
                    self.rwkv_r1()
                    self.rwkv_r2()
                    with self.scope():
                        self.XA = self.sb("XA", [128, KC, T])
                        P.dma('sp', self.XA[:], self.xa_scr)
                        self.rwkv_r3(2)
                        if self.dbg == ('mix', 2):
                            emit_out()
                        else:
                            self.HB = self.sb("HB", [128, KC, T], BF16)
                            ffn_layer(2, TILES)
                            if NL > 3:
                                tiles = TILES[1:]
                                self.norm_mod(3, 0, GM)
                                self.s5_mixer(1, 3, tiles)
                                ffn_layer(3, tiles)
                            emit_out()
            P.barrier()
            self.stats = (P.n_inst, P.n_wait, dict(P.cnt))
        return nc

    def adaln(self, ada_w, ada_b, cvec, NL):
        P = self.P
        with self.scope():
            SC = self.sb("SC", [128, KC, 2])
            ADB = self.sb("ADB", [128, 4, 48])
            WF = [self.sb("WF", [128, KC, 512]) for _ in range(2)]
            P.dma('sp', SC[:], cvec)
            P.dma('sp', ADB[:], ada_b)
            P.act(SC[:], SC[:], AF.Silu)
            i = 0
            for l in range(NL):
                for s in range(12):
                    wv = WF[i % 2]
                    i += 1
                    for half in range(2):
                        P.dma('sp' if half == 0 else 'act', wv[:, half * 4:(half + 1) * 4, :],
                              ada_w[l, half * 512:(half + 1) * 512, s * 512:(s + 1) * 512].rearrange("(k p) m -> p k m", p=128))
                    pb = self.bank()
                    for j in range(4):
                        for kk in range(KC):
                            P.mm(pb[:, j * 2:(j + 1) * 2], wv[:, kk, j * 128:(j + 1) * 128], SC[:, kk, :],
                                 start=(kk == 0), stop=(kk == KC - 1))
                    for j in range(4):
                        m = s * 4 + j
                        P.ts('dve', self.MOD[:, l, m, :], pb[:, j * 2:(j + 1) * 2], ADB[:, l, m:m + 1], ALU.add)

    def rstd_tile(self, t0, n, sq, rs, eps=1e-6):
        P = self.P
        pb = self.bank()
        for c in range(KC):
            P.act(sq[:, c, :n], self.XA[:, c, t0:t0 + n], AF.Square)
        for c in range(KC):
            P.mm(pb[:, :n], self.ones[:], sq[:, c, :n], start=(c == 0), stop=(c == KC - 1))
        P.act(rs[:, :n], pb[:, :n], AF.Sqrt, bias=eps, scale=1.0 / D)
        P.op('dve', lambda e: e.reciprocal(out=rs[:, :n], in_=rs[:, :n]), [rs[:, :n]], [rs[:, :n]])

    def norm_mod(self, l, which, G, want_router=False, fi=0):
        P = self.P
        XA, HB, MOD, AB = self.XA, self.HB, self.MOD, self.AB
        base = 0 if which == 0 else 3
        for w in range(2):
            P.ts('dve', AB[:, 0, :, w], MOD[:, l, (base + 1) * 8:(base + 2) * 8, w], 1.0, ALU.add)
            P.tt('dve', AB[:, 0, :, w], AB[:, 0, :, w], G[:, l, :], ALU.mult)
            P.copy('dve', AB[:, 1, :, w], MOD[:, l, base * 8:(base + 1) * 8, w])
        with self.scope():
            sq = self.sb("sq", [128, KC, 256])
            rs = self.sb("rs", [128, 256])
            tmp = [self.sb("tmp", [128, 256]) for _ in range(2)]
            if want_router:
                RT = self.sb("RT", [128, KC, NE])
                P.dma('sp', RT[:], self.moe_router[fi])
                RB = self.sb("RB", [128, KC, 128])
                hf = self.sb("hf", [128, KC, 256])
                E1 = self.sb("E1", [128, NE, 256])
                L2 = self.sb("L2", [128, NE, 256])
                M1 = self.sb("M1", [128, 256])
                M2 = self.sb("M2", [128, 256])
                P1 = self.sb("P1", [128, 256])
                P2 = self.sb("P2", [128, 256])
            for (t0, n, w) in TILES256:
                self.rstd_tile(t0, n, sq, rs)
                for c in range(KC):
                    tm = tmp[c % 2]
                    P.tt('dve', tm[:], XA[:, c, t0:t0 + n], rs[:], ALU.mult)
                    P.act(HB[:, c, t0:t0 + n], tm[:], AF.Identity, bias=AB[:, 1, c, w:w + 1], scale=AB[:, 0, c, w:w + 1])
                    if want_router:
                        P.ts('pool', hf[:, c, :], tm[:], AB[:, 0, c, w:w + 1], ALU.mult, AB[:, 1, c, w:w + 1], ALU.add)
                if not want_router:
                    continue
                banks = [self.bank(), self.bank(), self.bank(), self.bank()]
                for e in range(NE):
                    for k in range(KC):
                        P.ts('dve', RB[:, k, :], self.ones[:], RT[:, k, e:e + 1], ALU.mult)
                    pb = banks[e // 2]
                    for k in range(KC):
                        P.mm(pb[:, (e % 2) * 256:(e % 2) * 256 + n], RB[:, k, :], hf[:, k, :], start=(k == 0), stop=(k == KC - 1))
                def Lv(e):
                    return banks[e // 2][:, (e % 2) * 256:(e % 2) * 256 + n]
                P.copy('dve', M1[:], Lv(0))
                for e in range(1, NE):
                    P.tt('dve', M1[:], M1[:], Lv(e), ALU.max)
                for e in range(NE):
                    P.tt('dve', E1[:, e, :], Lv(e), M1[:], ALU.is_equal)
                    P.stt(L2[:, e, :], E1[:, e, :], -1e30, Lv(e), ALU.mult, ALU.add)
                P.copy('dve', M2[:], L2[:, 0, :])
                for e in range(1, NE):
                    P.tt('dve', M2[:], M2[:], L2[:, e, :], ALU.max)
                P.tt('dve', P2[:], M1[:], M2[:], ALU.subtract)
                P.act(P1[:], P2[:], AF.Sigmoid)
                P.act(P2[:], P2[:], AF.Sigmoid, scale=-1.0)
                for e in range(NE):
                    P.tt('dve', L2[:, e, :], L2[:, e, :], M2[:], ALU.is_equal)
                    P.tt('pool', L2[:, e, :], L2[:, e, :], P2[:], ALU.mult)
                    P.tt('pool', E1[:, e, :], E1[:, e, :], P1[:], ALU.mult)
                    P.tt('pool', E1[:, e, :], E1[:, e, :], L2[:, e, :], ALU.add)
                    P.dma('sp', self.gb_scr[e, :, t0:t0 + n], E1[:, e, :])

    def glu_linear(self, W, bias_fn, n_out_chunks, src, tiles, consume, glu=True, after_chunk=None):
        P = self.P
        with self.scope():
            WS = [self.sb("WS", [128, 2, KC, 128], BF16) for _ in range(2)]
            RES = [self.sb("RES", [128, 512]) for _ in range(2)]
            SG = [self.sb("SG", [128, 512]) for _ in range(2)]
            it = 0
            for j in range(n_out_chunks):
                wa = WS[j % 2]
                P.dma('pool', wa[:, 0], W[:, j * 128:(j + 1) * 128].rearrange("(k p) m -> p k m", p=128))
                if glu:
                    P.dma('pool', wa[:, 1], W[:, D + j * 128:D + (j + 1) * 128].rearrange("(k p) m -> p k m", p=128))
                for (t0, n, w) in tiles:
                    pa = self.bank()
                    for k in range(KC):
                        P.mm(pa[:, :n], wa[:, 0, k, :], src[:, k, t0:t0 + n], start=(k == 0), stop=(k == KC - 1))
                    res = RES[it % 2][:, :n]
                    if glu:
                        pg = self.bank()
                        for k in range(KC):
                            P.mm(pg[:, :n], wa[:, 1, k, :], src[:, k, t0:t0 + n], start=(k == 0), stop=(k == KC - 1))
                        sg = SG[it % 2][:, :n]
                        P.act(sg, pg[:, :n], AF.Sigmoid, bias=bias_fn(8 + j))
                        P.stt(res, pa[:, :n], bias_fn(j), sg, ALU.add, ALU.mult)
                    else:
                        P.ts('dve', res, pa[:, :n], bias_fn(j), ALU.add)
                    it += 1
                    consume(j, t0, n, w, res)
                if after_chunk is not None:
                    after_chunk(j)

    def resid_add(self, l, gate_idx):
        P = self.P
        XA, MOD = self.XA, self.MOD

        def consume(j, t0, n, w, res):
            g = MOD[:, l, gate_idx * 8 + j, w:w + 1]
            P.stt(XA[:, j, t0:t0 + n], res, g, XA[:, j, t0:t0 + n], ALU.mult, ALU.add)
        return consume

    def sincos(self, ang, sn, cs, ki, n):
        P = self.P
        P.ts('pool', ki, ang, 1.0 / TWO_PI, ALU.mult)
        P.copy('pool', cs, ki)
        P.ts('pool', cs, cs, -TWO_PI, ALU.mult)
        P.tt('pool', ang, ang, cs, ALU.add)
        P.ts('pool', cs, ang, math.pi, ALU.is_gt, -TWO_PI, ALU.mult)
        P.tt('pool', ang, ang, cs, ALU.add)
        P.ts('pool', cs, ang, -math.pi, ALU.is_lt, TWO_PI, ALU.mult)
        P.tt('pool', ang, ang, cs, ALU.add)
        P.ts('dve', ang, ang, PI_C, ALU.min, -PI_C, ALU.max)
        P.act(sn, ang, AF.Sin)
        P.act(ang, ang, AF.Abs)
        P.act(cs, ang, AF.Sin, bias=self.halfpi[:, 0:1], scale=-1.0)

    def s5_mixer(self, j, l, tiles):
        P = self.P
        XA, HB = self.XA, self.HB
        with self.scope():
            halfpi = self.sb("halfpi", [128, 1])
            self.halfpi = halfpi
            P.memset('dve', halfpi[:], math.pi / 2)
            PAR = self.sb("PAR", [128, 2, 3, 32])
            RHO = self.sb("RHO", [128, 2, 32])
            TH = self.sb("TH", [128, 2, 32])
            GR = self.sb("GR", [128, 2, 32])
            GI = self.sb("GI", [128, 2, 32])
            A1 = self.sb("A1", [128, 2, 32])
            A2 = self.sb("A2", [128, 2, 32])
            A3 = self.sb("A3", [128, 2, 32])
            A4 = self.sb("A4", [128, 2, 32])
            KI0 = self.sb("KI0", [128, 2, 32], I32)
            DV = self.sb("DV", [128, KC])
            GB = self.sb("GLUB", [128, 16])
            P.dma('sp', DV[:], self.s5_d[:, j, :])
            P.dma('sp', GB[:], self.s5_glu_b[:, j, :])
            for d in range(2):
                P.dma('sp', PAR[:, d], self.s5_par[j, d])
            lr = A1
            P.ts('dve', lr[:], PAR[:, :, 0, :], -1e-4, ALU.min)
            P.act(A2[:], PAR[:, :, 2, :], AF.Exp)
            P.tt('dve', RHO[:], lr[:], A2[:], ALU.mult)
            P.act(RHO[:], RHO[:], AF.Exp)
            P.tt('dve', TH[:], PAR[:, :, 1, :], A2[:], ALU.mult)
            P.copy('dve', A3[:], TH[:])
            sn0, cs0 = A2, A4
            self.sincos(A3[:].rearrange("p a b -> p (a b)"), sn0[:].rearrange("p a b -> p (a b)"),
                        cs0[:].rearrange("p a b -> p (a b)"), KI0[:].rearrange("p a b -> p (a b)"), 64)
            P.tt('dve', A3[:], RHO[:], cs0[:], ALU.mult)
            P.ts('dve', A3[:], A3[:], -1.0, ALU.add)
            P.tt('dve', A2[:], RHO[:], sn0[:], ALU.mult)
            li = PAR[:, :, 1, :]
            P.tt('dve', A4[:], lr[:], lr[:], ALU.mult)
            P.tt('dve', GR[:], li, li, ALU.mult)
            P.tt('dve', A4[:], A4[:], GR[:], ALU.add)
            P.op('dve', lambda e: e.reciprocal(out=A4[:], in_=A4[:]), [A4[:]], [A4[:]])
            P.tt('dve', GR[:], A3[:], lr[:], ALU.mult)
            P.tt('dve', GI[:], A2[:], li, ALU.mult)
            P.tt('dve', GR[:], GR[:], GI[:], ALU.add)
            P.tt('dve', GR[:], GR[:], A4[:], ALU.mult)
            P.tt('dve', GI[:], A2[:], lr[:], ALU.mult)
            P.tt('dve', A2[:], A3[:], li, ALU.mult)
            P.tt('dve', GI[:], GI[:], A2[:], ALU.subtract)
            P.tt('dve', GI[:], GI[:], A4[:], ALU.mult)

            IOI = self.sb("IOI", [128, 512], I32)
            IOTA = self.sb("IOTA", [128, T])
            for (t0, n, w) in TILES:
                P.op('pool', lambda e: e.iota(IOI[:, :n], [[1, n]], base=t0, channel_multiplier=0), [], [IOI[:, :n]])
                P.copy('dve', IOTA[:, t0:t0 + n], IOI[:, :n])

            BW = self.sb("BW", [128, 8, 2, 128], BF16)
            CF = [self.sb("CF", [128, 2, 128]) for _ in range(2)]
            CW = self.sb("CW", [128, 8, 2, 128], BF16)
            NS = 14
            SL = [self.sb("SL", [128, 512]) for _ in range(NS)]
            KI = self.sb("KI", [128, 512], I32)
            HRI = [self.sb("HRI", [128, 2, 512], BF16) for _ in range(2)]
            RHOB = self.sb("RHOB", [128, 512])
            self.sl_i = 0

            def slot():
                s = SL[self.sl_i]
                self.sl_i = (self.sl_i + 1) % NS
                return s

            ybank = self.ps[0:5]
            pri = [self.ps[5], self.ps[6]]
            it = 0
            for cch in range(KC):
                for scl in range(4):
                    sc = cch * 4 + scl
                    for d in range(2):
                        q = scl * 2 + d
                        P.dma('pool', BW[:, q], self.s5_B[j, d, sc])
                        cf = CF[q % 2]
                        P.dma('sp', cf[:], self.s5_C[j, d, sc])
                        t = slot()
                        P.ts('dve', t[:, 0:128], cf[:, 1, :], GI[:, d, sc:sc + 1], ALU.mult)
                        P.stt(CW[:, q, 0, :], cf[:, 0, :], GR[:, d, sc:sc + 1], t[:, 0:128], ALU.mult, ALU.subtract)
                        P.ts('dve', t[:, 128:256], cf[:, 1, :], GR[:, d, sc:sc + 1], ALU.mult)
                        P.stt(t[:, 256:384], cf[:, 0, :], GI[:, d, sc:sc + 1], t[:, 128:256], ALU.mult, ALU.add)
                        P.ts('dve', CW[:, q, 1, :], t[:, 256:384], -1.0, ALU.mult)
                for scl in range(4):
                    sc = cch * 4 + scl
                    for d in range(2):
                        q = scl * 2 + d
                        P.ts('dve', RHOB[:], IOTA[:, 0:512], 0.0, ALU.mult, RHO[:, d, sc:sc + 1], ALU.add)
                        carry = [0.0, 0.0]
                        for ti, (s0, n, w) in enumerate(TILES):
                            if d == 0:
                                nat = ti
                                uview = HB[:, cch, s0:s0 + n]
                            else:
                                nat = 0 if ti == 0 else 5 - ti
                                hi = 255 if ti == 0 else 2303 - 512 * (ti - 1)
                                lo = hi - n + 1
                                uview = HB[:, cch, hi::-1] if lo == 0 else HB[:, cch, hi:lo - 1:-1]
                            ang, sn, cs = slot(), slot(), slot()
                            P.ts('pool', ang[:, :n], IOTA[:, s0:s0 + n], TH[:, d, sc:sc + 1], ALU.mult)
                            self.sincos(ang[:, :n], sn[:, :n], cs[:, :n], KI[:, :n], n)
                            P.mm(pri[0][:, :n], BW[:, q, 0, :], uview, start=True, stop=True)
                            P.mm(pri[1][:, :n], BW[:, q, 1, :], uview, start=True, stop=True)
                            t1, t2, gr, gi = slot(), slot(), slot(), slot()
                            P.tt('dve', t1[:, :n], cs[:, :n], pri[0][:, :n], ALU.mult)
                            P.tt('dve', t2[:, :n], sn[:, :n], pri[1][:, :n], ALU.mult)
                            P.tt('pool', gr[:, :n], t1[:, :n], t2[:, :n], ALU.add)
                            P.tt('dve', t1[:, :n], cs[:, :n], pri[1][:, :n], ALU.mult)
                            P.tt('dve', t2[:, :n], sn[:, :n], pri[0][:, :n], ALU.mult)
                            P.tt('pool', gi[:, :n], t1[:, :n], t2[:, :n], ALU.subtract)
                            for z, g in enumerate((gr, gi)):
                                init = carry[z]
                                P.op('dve', lambda e, g=g, init=init: e.tensor_tensor_scan(
                                    out=g[:, :n], data0=RHOB[:, :n], data1=g[:, :n], initial=init, op0=ALU.mult, op1=ALU.add),
                                    [RHOB[:, :n], g[:, :n]] + ([] if isinstance(init, float) else [init]), [g[:, :n]])
                                carry[z] = g[:, n - 1:n]
                            hri = HRI[it % 2]
                            it += 1
                            t3, t4 = slot(), slot()
                            P.tt('pool', t3[:, :n], cs[:, :n], gr[:, :n], ALU.mult)
                            P.tt('pool', t4[:, :n], sn[:, :n], gi[:, :n], ALU.mult)
                            P.tt('dve', hri[:, 0, :n], t3[:, :n], t4[:, :n], ALU.subtract)
                            t5, t6 = slot(), slot()
                            P.tt('pool', t5[:, :n], sn[:, :n], gr[:, :n], ALU.mult)
                            P.tt('pool', t6[:, :n], cs[:, :n], gi[:, :n], ALU.mult)
                            P.tt('dve', hri[:, 1, :n], t5[:, :n], t6[:, :n], ALU.add)
                            first = (scl == 0 and d == 0)
                            lastq = (scl == 3 and d == 1)
                            for z in range(2):
                                rv = hri[:, z, :n] if d == 0 else hri[:, z, n - 1::-1]
                                P.mm(ybank[nat][:, :n], CW[:, q, z, :], rv, start=(first and z == 0), stop=(lastq and z == 1))
                for ti, (t0, n, w) in enumerate(TILES):
                    v, u2 = slot(), slot()
                    P.stt(v[:, :n], HB[:, cch, t0:t0 + n], DV[:, cch:cch + 1], ybank[ti][:, :n], ALU.mult, ALU.add)
                    P.tt('pool', u2[:, :n], v[:, :n], v[:, :n], ALU.mult)
                    P.ts('pool', u2[:, :n], u2[:, :n], 0.044715, ALU.mult, 1.0, ALU.add)
                    P.tt('pool', u2[:, :n], u2[:, :n], v[:, :n], ALU.mult)
                    P.act(u2[:, :n], u2[:, :n], AF.Sigmoid, scale=2.0 * math.sqrt(2.0 / math.pi))
                    P.tt('dve', HB[:, cch, t0:t0 + n], v[:, :n], u2[:, :n], ALU.mult)
        with self.scope():
            GB = self.sb("GLUB2", [128, 16])
            P.dma('sp', GB[:], self.s5_glu_b[:, j, :])
            self.glu_linear(self.s5_glu_w[j], lambda i: GB[:, i:i + 1], KC, HB, tiles, self.resid_add(l, 2), glu=True)

    def conv_mixer(self, l, tiles):
        P = self.P
        XA, HB = self.XA, self.HB
        with self.scope():
            PB = self.sb("PB", [128, 16])
            DW = self.sb("DW", [128, KC, 31])
            CV = self.sb("CV", [128, 4, KC])
            P.dma('sp', PB[:], self.cv_pw1_b)
            P.dma('sp', DW[:], self.cv_dw_w)
            P.dma('sp', CV[:], self.cv_vec)
            UJ = [self.sb("UJ", [128, T]) for _ in range(2)]
            VJ = [self.sb("VJ", [128, T]) for _ in range(2)]

            def consume(j, t0, n, w, res):
                P.copy('pool', UJ[j % 2][:, t0:t0 + n], res)

            def after_chunk(j):
                u, v = UJ[j % 2], VJ[j % 2]
                P.ts('dve', v[:], u[:], DW[:, j, 15:16], ALU.mult, CV[:, 0, j:j + 1], ALU.add)
                for tap in range(31):
                    o = tap - 15
                    if o == 0:
                        continue
                    a = max(0, -o)
                    b = 64 - max(0, o)
                    if not (l == 3):
                        ac, bc = max(0, -o), 256 - max(0, o)
                        P.stt(v[:, ac:bc], u[:, ac + o:bc + o], DW[:, j, tap:tap + 1], v[:, ac:bc], ALU.mult, ALU.add)
                    vv = v[:, LC:].rearrange("p (r w) -> p r w", w=64)
                    uu = u[:, LC:].rearrange("p (r w) -> p r w", w=64)
                    P.stt(vv[:, :, a:b], uu[:, :, a + o:b + o], DW[:, j, tap:tap + 1], vv[:, :, a:b], ALU.mult, ALU.add)
                P.dma('sp', self.v_scr[j * 128:(j + 1) * 128, :], v[:])

            self.glu_linear(self.cv_pw1_w[0], lambda i: PB[:, i:i + 1], KC, HB, TILES, consume, glu=True, after_chunk=after_chunk)
            VT = [self.sb("VT", [128, KC, 256]) for _ in range(2)]
            SQ = self.sb("SQ", [128, KC, 256])
            MEAN = self.sb("MEAN", [128, 256])
            RSTD = self.sb("RSTD", [128, 256])
            TM = [self.sb("TM", [128, 256]) for _ in range(2)]
            GBV = self.sb("GBV", [128, KC])
            for ti, (t0, n, w) in enumerate(TILES256):
                vt = VT[ti % 2]
                P.dma('sp', vt[:], self.v_scr[:, t0:t0 + n].rearrange("(c p) n -> p c n", p=128))
                p1, p2 = self.bank(), self.bank()
                for c in range(KC):
                    P.act(SQ[:, c, :], vt[:, c, :], AF.Square)
                for c in range(KC):
                    P.mm(p1[:, :n], self.ones[:], vt[:, c, :], start=(c == 0), stop=(c == KC - 1))
                for c in range(KC):
                    P.mm(p2[:, :n], self.ones[:], SQ[:, c, :], start=(c == 0), stop=(c == KC - 1))
                P.ts('dve', MEAN[:], p1[:, :n], 1.0 / D, ALU.mult)
                P.tt('dve', RSTD[:], MEAN[:], MEAN[:], ALU.mult)
                P.stt(RSTD[:], p2[:, :n], 1.0 / D, RSTD[:], ALU.mult, ALU.subtract)
                P.act(RSTD[:], RSTD[:], AF.Sqrt, bias=1e-5)
                P.op('dve', lambda e: e.reciprocal(out=RSTD[:], in_=RSTD[:]), [RSTD[:]], [RSTD[:]])
                for c in range(KC):
                    tm = TM[c % 2]
                    P.tt('dve', tm[:], vt[:, c, :], MEAN[:], ALU.subtract)
                    P.tt('pool', tm[:], tm[:], RSTD[:], ALU.mult)
                    P.act(HB[:, c, t0:t0 + n], tm[:], AF.Silu, bias=CV[:, 2, c:c + 1], scale=CV[:, 1, c:c + 1])
            self.glu_linear(self.cv_pw2_w[0], lambda i: CV[:, 3, i:i + 1], KC, HB, tiles, self.resid_add(l, 2), glu=False)


    def rwkv_decl(self):
        nc = self.nc
        self.rw_mu = self.inp("rw_mu", [128, 6, KC])
        self.rw_w = {k: self.inp("rw_w_" + k, [D, D]) for k in ("r", "k", "v")}
        self.rw_wo = self.inp("rw_w_o", [64, 16, D])
        self.rw_w1 = self.inp("rw_w1", [2, D, 64])
        self.rw_w2 = self.inp("rw_w2", [2, 64, D])
        self.rw_a1 = self.inp("rw_a1", [2, D, 64])
        self.rw_a2 = self.inp("rw_a2", [2, 64, D])
        self.rw_g1 = self.inp("rw_g1", [D, 160])
        self.rw_g2 = self.inp("rw_g2", [160, D])
        self.rw_vec = self.inp("rw_vec", [64, 9, 16])
        self.rs_ = {}
        for d in range(2):
            for nm in ("sg", "a", "r", "k", "y", "kd"):
                self.rs_[(nm, d)] = self.scratch("rws_%s%d" % (nm, d), [64, 16, T])
            self.rs_[("V", d)] = self.scratch("rws_V%d" % d, [T, D])
        self.rs_["vT"] = self.scratch("rws_vT", [64, 16, T])
        self.rs_["g"] = self.scratch("rws_g", [64, 16, T])
        self.xa_scr = self.scratch("xa_scr", [128, KC, T])

    def rwkv_mixer(self, l, tiles):
        P = self.P
        XA, HB = self.XA, self.HB
        P.dma('sp', self.xa_scr, XA[:])
        P.barrier()
        self.rwkv_r1()
        self.rwkv_r2()
        P.dma('sp', XA[:], self.xa_scr)
        self.rwkv_r3(l)

    def rwkv_r1(self):
        P = self.P
        with self.scope():
            HB = self.sb("HBr", [128, KC, T], BF16)
            P.dma('sp', HB[:], self.hb_scr)
            XX = self.sb("XX", [128, KC, T], BF16)
            XV = self.sb("XV", [128, KC, T], BF16)
            MU = self.sb("MU", [128, 6, KC])
            VEC = self.sb("VEC", [64, 9, 16])
            P.dma('sp', MU[:], self.rw_mu)
            P.dma('sp', VEC[:], self.rw_vec)
            WS = [self.sb("WS", [128, KC, 128], BF16) for _ in range(2)]
            WV = self.sb("WV", [128, KC, D], BF16)
            W1 = self.sb("W1", [128, KC, 64], BF16)
            W2 = self.sb("W2", [64, D], BF16)
            G1 = self.sb("G1", [128, KC, 160], BF16)
            G2a = self.sb("G2a", [128, D], BF16)
            G2b = self.sb("G2b", [32, D], BF16)
            H1 = [self.sb("H1", [64, 512], BF16) for _ in range(2)]
            HG0 = self.sb("HG0", [128, 512], BF16)
            HG1 = self.sb("HG1", [32, 512], BF16)
            OT = [self.sb("OT", [128, 512]) for _ in range(4)]
            self.ot_i = 0

            def ot():
                o = OT[self.ot_i % 4]
                self.ot_i += 1
                return o

            parts = [(0, LC), (LC, T)]
            for d in range(2):
                if d == 1:
                    for c in range(KC):
                        P.copy('dve' if c % 2 == 0 else 'pool', XV[:, c, 0:LC], HB[:, c, LC - 1::-1])
                        P.copy('dve' if c % 2 == 0 else 'pool', XV[:, c, LC:T], HB[:, c, T - 1:LC - 1:-1])
                    for c in range(KC):
                        P.copy('pool' if c % 2 == 0 else 'dve', HB[:, c, :], XV[:, c, :])
                for c in range(KC):
                    P.ts('pool', XX[:, c, :], HB[:, c, :], -1.0, ALU.mult)
                for (a, b) in parts:
                    for c in range(KC):
                        P.stt(XX[:, c, a + 1:b], HB[:, c, a:b - 1], 0.5, XX[:, c, a + 1:b], ALU.mult, ALU.add)
                        P.stt(XX[:, c, a:b - 1], HB[:, c, a + 1:b], 0.5, XX[:, c, a:b - 1], ALU.mult, ALU.add)

                def variant(m):
                    for c in range(KC):
                        P.stt(XV[:, c, :], XX[:, c, :], MU[:, m, c:c + 1], HB[:, c, :], ALU.mult, ALU.add)

                def head_proj(Wd, dst):
                    for hp in range(8):
                        wa = WS[hp % 2]
                        P.dma('pool', wa[:], Wd[:, hp * 128:(hp + 1) * 128].rearrange("(k p) m -> p k m", p=128))
                        for (t0, n, w) in TILES:
                            for hh in range(2):
                                h = hp * 2 + hh
                                pa = self.bank()
                                for k in range(KC):
                                    P.mm(pa[0:64, :n], wa[:, k, hh * 64:(hh + 1) * 64], XV[:, k, t0:t0 + n],
                                         start=(k == 0), stop=(k == KC - 1))
                                o = ot()
                                P.copy('act' if h % 2 == 0 else 'dve', o[0:64, :n], pa[0:64, :n])
                                P.dma('sp', dst[:, h, t0:t0 + n], o[0:64, :n])

                def lora(w1d, w2d, bias_idx, func1, dst):
                    P.dma('pool', W1[:], w1d.rearrange("(k p) m -> p k m", p=128))
                    P.dma('pool', W2[:], w2d)
                    for ti, (t0, n, w) in enumerate(TILES):
                        p1 = self.bank()
                        for k in range(KC):
                            P.mm(p1[0:64, :n], W1[:, k, :], XV[:, k, t0:t0 + n], start=(k == 0), stop=(k == KC - 1))
                        h1 = H1[ti % 2]
                        P.act(h1[:, :n], p1[0:64, :n], func1)
                        for h in range(16):
                            p2 = self.bank()
                            P.mm(p2[0:64, :n], W2[:, h * 64:(h + 1) * 64], h1[:, :n], start=True, stop=True)
                            o = ot()
                            P.act(o[0:64, :n], p2[0:64, :n], AF.Sigmoid, bias=VEC[:, bias_idx, h:h + 1])
                            P.dma('sp', dst[:, h, t0:t0 + n], o[0:64, :n])

                variant(0)
                head_proj(self.rw_w["r"], self.rs_[("r", d)])
                variant(2)
                head_proj(self.rw_w["k"], self.rs_[("k", d)])
                variant(3)
                if d == 0:
                    head_proj(self.rw_w["v"], self.rs_["vT"])
                P.dma('pool', WV[:], self.rw_w["v"].rearrange("(k p) m -> p k m", p=128))
                for tt in range(T // 128):
                    for half in range(2):
                        pv = self.bank()
                        for k in range(KC):
                            P.mm(pv[:, :], XV[:, k, tt * 128:(tt + 1) * 128], WV[:, k, half * 512:(half + 1) * 512],
                                 start=(k == 0), stop=(k == KC - 1))
                        o = ot()
                        P.copy('act' if half == 0 else 'dve', o[:, :], pv[:, :])
                        P.dma('sp', self.rs_[("V", d)][tt * 128:(tt + 1) * 128, half * 512:(half + 1) * 512], o[:, :])
                variant(1)
                lora(self.rw_w1[d], self.rw_w2[d], 0 + d, AF.Tanh, self.rs_[("sg", d)])
                variant(4)
                lora(self.rw_a1[d], self.rw_a2[d], 2 + d, AF.Copy, self.rs_[("a", d)])
                if d == 0:
                    variant(5)
                    P.dma('pool', G1[:], self.rw_g1.rearrange("(k p) m -> p k m", p=128))
                    P.dma('pool', G2a[:], self.rw_g2[0:128, :])
                    P.dma('pool', G2b[:], self.rw_g2[128:160, :])
                    for (t0, n, w) in TILES:
                        p0, p1 = self.bank(), self.bank()
                        for k in range(KC):
                            P.mm(p0[:, :n], G1[:, k, 0:128], XV[:, k, t0:t0 + n], start=(k == 0), stop=(k == KC - 1))
                        for k in range(KC):
                            P.mm(p1[0:32, :n], G1[:, k, 128:160], XV[:, k, t0:t0 + n], start=(k == 0), stop=(k == KC - 1))
                        P.act(HG0[:, :n], p0[:, :n], AF.Sigmoid)
                        P.act(HG1[:, :n], p1[0:32, :n], AF.Sigmoid)
                        for h in range(16):
                            p2 = self.bank()
                            P.mm(p2[0:64, :n], G2a[:, h * 64:(h + 1) * 64], HG0[:, :n], start=True, stop=False)
                            P.mm(p2[0:64, :n], G2b[:, h * 64:(h + 1) * 64], HG1[:, :n], start=False, stop=True)
                            o = ot()
                            P.copy('dve', o[0:64, :n], p2[0:64, :n])
                            P.dma('sp', self.rs_["g"][:, h, t0:t0 + n], o[0:64, :n])

    def rwkv_r2(self):
        P = self.P
        L = 64
        NCH = T // L
        with self.scope():
            def tl(name):
                return self.sb(name, [64, 16, 64])
            VEC = self.sb("VEC2", [64, 9, 16])
            P.dma('sp', VEC[:], self.rw_vec)
            ones64 = self.sb("ones64", [64, 64])
            P.memset('dve', ones64[:], 1.0)
            ident = self.sb("ident", [64, 64])
            mU = self.sb("mU", [64, 64])
            mUi = self.sb("mUi", [64, 64])
            mL = self.sb("mL", [64, 64])
            for (m, pat, op, cm) in ((mU, 1, ALU.is_gt, -1), (mUi, 1, ALU.is_ge, -1), (mL, -1, ALU.is_gt, 1), (ident, 1, ALU.is_equal, -1)):
                P.op('pool', lambda e, m=m, pat=pat, op=op, cm=cm: e.affine_select(
                    out=m[:], in_=ones64[:], pattern=[[pat, 64]], compare_op=op, fill=0.0, base=0, channel_multiplier=cm),
                    [ones64[:]], [m[:]])
            rmask = tl("rmask")
            P.memset('dve', rmask[:], 1.0)
            P.memset('dve', rmask[:, :, 0:1], 0.0)
            LD = [[tl("ld%d_%d" % (i, s)) for i in range(4)] for s in range(2)]
            VT = [self.sb("Vt%d" % s, [64, 16, 64]) for s in range(2)]
            At, Rt, Bt, Kt, BhT, KhT, Bh, Kh = [tl(n) for n in ("At", "Rt", "Bt", "Kt", "BhT", "KhT", "Bh", "Kh")]
            lp, tA, tB, tC, kd = [tl(n) for n in ("lp", "tA", "tB", "tC", "kd")]
            PL = self.sb("PL", [64, 16])
            NN = [[tl("N%d_%d" % (i, s)) for i in range(2)] for s in range(2)]
            QT = [tl("QT0"), tl("QT1")]
            AkT, ArbT, ArkT, Wt, Ut, Yc, ST = [tl(n) for n in ("AkT", "ArbT", "ArkT", "Wt", "Ut", "Yc", "ST")]

            def bc_t(v):
                return v.unsqueeze(2).to_broadcast([64, 16, 64])

            def bc_h(m, nh=8):
                return m.unsqueeze(1).to_broadcast([64, nh, 64])

            def stage(fn_mm, evac):
                for hb in range(2):
                    b = self.bank()
                    for hh in range(8):
                        h = hb * 8 + hh
                        fn_mm(h, b[0:64, hh * 64:(hh + 1) * 64])
                    evac(b[0:64, :].rearrange("p (h t) -> p h t", h=8), slice(hb * 8, hb * 8 + 8), hb)

            for d in range(2):
                P.memset('dve', ST[:], 0.0)
                srcs = [self.rs_[(nm, d)] for nm in ("sg", "a", "r", "k")]

                def load(c):
                    s = c % 2
                    for i in range(4):
                        P.dma('sp' if i % 2 == 0 else 'act', LD[s][i][:], srcs[i][:, :, c * L:(c + 1) * L])
                    P.dma('act', VT[s][:], self.rs_[("V", d)][c * L:(c + 1) * L, :].rearrange("s (h i) -> s h i", h=16))

                load(0)
                for c in range(NCH):
                    if c + 1 < NCH:
                        load(c + 1)
                    sg, a, r, k = LD[c % 2]
                    V = VT[c % 2]
                    P.ts('dve', tA[:], sg[:], -0.6065306597126334, ALU.mult)
                    P.op('dve', lambda e: e.tensor_tensor_scan(
                        out=lp[:].rearrange("p h t -> p (h t)"), data0=rmask[:].rearrange("p h t -> p (h t)"),
                        data1=tA[:].rearrange("p h t -> p (h t)"), initial=0.0, op0=ALU.mult, op1=ALU.add),
                        [rmask[:], tA[:]], [lp[:]])
                    P.act(PL[:], lp[:, :, L - 1], AF.Exp)
                    P.tt('pool', tA[:], lp[:], tA[:], ALU.subtract)
                    P.act(tA[:], tA[:], AF.Exp)
                    P.tt('pool', tB[:], k[:], bc_t(VEC[:, 4, :]), ALU.mult)
                    P.tt('pool', tC[:], tB[:], tB[:], ALU.mult)

                    def mm_ss(h, out):
                        P.mm(out, ones64[:], tC[:, h, :], True, True)

                    def ev_ss(bv, hs, hb):
                        P.act(tC[:, hs, :], bv, AF.Sqrt, bias=VEC[:, 4, 0:1] if False else 1e-12)
                    stage(mm_ss, ev_ss)
                    P.op('dve', lambda e: e.reciprocal(out=tC[:], in_=tC[:]), [tC[:]], [tC[:]])
                    P.tt('dve', tB[:], tB[:], tC[:], ALU.mult)
                    P.stt(At[:], tB[:], -1.0, tA[:], ALU.mult, ALU.mult)
                    P.act(tA[:], lp[:], AF.Exp, scale=-1.0)
                    P.tt('pool', tB[:], tB[:], a[:], ALU.mult)
                    P.tt('pool', Bt[:], tB[:], tA[:], ALU.mult)
                    P.ts('pool', tC[:], a[:], -1.0, ALU.add)
                    P.tt('pool', tC[:], tC[:], bc_t(VEC[:, 5, :]), ALU.mult)
                    P.stt(kd[:], tC[:], 1.0, k[:], ALU.add, ALU.mult)
                    P.dma('sp', self.rs_[("kd", d)][:, :, c * L:(c + 1) * L], kd[:])
                    P.tt('pool', Kt[:], kd[:], tA[:], ALU.mult)
                    P.act(tC[:], lp[:], AF.Exp)
                    P.tt('dve', Rt[:], r[:], tC[:], ALU.mult)
                    P.tt('pool', BhT[:], Bt[:], bc_t(PL[:]), ALU.mult)
                    P.tt('pool', KhT[:], Kt[:], bc_t(PL[:]), ALU.mult)
                    N0, NT0 = NN[0]

                    def mk(lh, rh):
                        def f(h, out):
                            P.mm(out, lh[:, h, :], rh[:, h, :], True, True)
                        return f

                    def ev_mask(dst, mask):
                        def f(bv, hs, hb):
                            P.tt('dve', dst[:, hs, :], bv, bc_h(mask[:]), ALU.mult)
                        return f

                    def ev_copy(dst, eng):
                        def f(bv, hs, hb):
                            P.copy(eng if hb == 0 else ('dve' if eng == 'act' else 'act'), dst[:, hs, :], bv)
                        return f
                    stage(mk(At, Bt), ev_mask(N0, mL))
                    stage(mk(Bt, At), ev_mask(NT0, mU))
                    stage(mk(Kt, At), ev_mask(AkT, mU))
                    stage(mk(Bt, Rt), ev_mask(ArbT, mUi))
                    stage(mk(Kt, Rt), ev_mask(ArkT, mUi))

                    def tr(src):
                        def f(h, out):
                            P.op('pe', lambda e: e.transpose(out, src[:, h, :], ident[:]), [src[:, h, :], ident[:]], [out])
                        return f
                    stage(tr(BhT), ev_copy(Bh, 'act'))
                    stage(tr(KhT), ev_copy(Kh, 'act'))
                    q = 0
                    P.tt('pool', QT[0][:], NT0[:], bc_h(ident[:], 16), ALU.add)
                    cur = 0
                    for m in range(5):
                        Np, NpT = NN[cur]
                        Nn, NnT = NN[1 - cur]
                        stage(mk(NpT, Np), ev_copy(Nn, 'act'))
                        if m < 4:
                            stage(mk(Np, NpT), ev_copy(NnT, 'dve'))
                        qo, qn = QT[q], QT[1 - q]

                        def ev_add(bv, hs, hb, qo=qo, qn=qn):
                            P.tt('dve', qn[:, hs, :], bv, qo[:, hs, :], ALU.add)
                        stage(mk(Nn, qo), ev_add)
                        q = 1 - q
                        cur = 1 - cur
                    XT = QT[q]

                    def mm_W(h, out):
                        P.mm(out, At[:, h, :], ST[:, h, :], True, False)
                        P.mm(out, AkT[:, h, :], V[:, h, :], False, True)
                    stage(mm_W, ev_copy(Wt, 'act'))
                    stage(mk(XT, Wt), ev_copy(Ut, 'act'))

                    def mm_Y(h, out):
                        P.mm(out, ST[:, h, :], Rt[:, h, :], True, False)
                        P.mm(out, Ut[:, h, :], ArbT[:, h, :], False, False)
                        P.mm(out, V[:, h, :], ArkT[:, h, :], False, True)
                    stage(mm_Y, ev_copy(Yc, 'act'))
                    P.dma('sp', self.rs_[("y", d)][:, :, c * L:(c + 1) * L], Yc[:])

                    def mm_S(h, out):
                        P.mm(out, Bh[:, h, :], Ut[:, h, :], True, False)
                        P.mm(out, Kh[:, h, :], V[:, h, :], False, True)

                    def ev_S(bv, hs, hb):
                        P.tt('pool', ST[:, hs, :], ST[:, hs, :], PL[:, hs].unsqueeze(2).to_broadcast([64, 8, 64]), ALU.mult)
                        P.tt('dve', ST[:, hs, :], ST[:, hs, :], bv, ALU.add)
                    stage(mm_S, ev_S)

    def rwkv_r3(self, l):
        P = self.P
        XA, MOD = self.XA, self.MOD
        n = 128
        with self.scope():
            VEC = self.sb("VEC3", [64, 9, 16])
            P.dma('sp', VEC[:], self.rw_vec)
            ones64 = self.sb("ones64b", [64, 64])
            P.memset('dve', ones64[:], 1.0)
            WO = self.sb("WO", [64, 16, D], BF16)
            P.dma('pool', WO[:], self.rw_wo)

            def tl(name, dt=F32):
                return self.sb(name, [64, 16, n], dt)
            yf, yb, kf, kb, rr, vT, gg, t1, t2 = [tl(x) for x in ("yf", "yb", "kf", "kb", "rr", "vT", "gg", "t1", "t2")]
            Z = tl("Z", BF16)

            def bc_t(v):
                return v.unsqueeze(2).to_broadcast([64, 16, n])

            def ones_mm(src, evac):
                for q4 in range(4):
                    b = self.bank()
                    P.mm(b[0:64, :], ones64[:], src[:, q4 * 4:(q4 + 1) * 4, :].rearrange("p h t -> p (h t)"), True, True)
                    evac(b[0:64, :].rearrange("p (h t) -> p h t", h=4), slice(q4 * 4, q4 * 4 + 4))

            for tt in range(T // n):
                t0 = tt * n
                if t0 < LC:
                    s_hi = LC - 1 - t0
                else:
                    s_hi = (T - 1) - (t0 - LC)
                s_lo = s_hi - n + 1
                P.dma('sp', yf[:], self.rs_[("y", 0)][:, :, t0:t0 + n])
                P.dma('act', yb[:], self.rs_[("y", 1)][:, :, s_lo:s_hi + 1])
                P.dma('sp', kf[:], self.rs_[("kd", 0)][:, :, t0:t0 + n])
                P.dma('act', kb[:], self.rs_[("kd", 1)][:, :, s_lo:s_hi + 1])
                P.dma('sp', rr[:], self.rs_[("r", 0)][:, :, t0:t0 + n])
                P.dma('act', vT[:], self.rs_["vT"][:, :, t0:t0 + n])
                P.dma('sp', gg[:], self.rs_["g"][:, :, t0:t0 + n])
                P.tt('dve', yf[:], yf[:], yb[:, :, ::-1], ALU.add)
                ones_mm(yf, lambda bv, hs: P.stt(yf[:, hs, :], bv, -1.0 / 64, yf[:, hs, :], ALU.mult, ALU.add))
                P.tt('pool', t1[:], yf[:], yf[:], ALU.mult)

                def ev_var(bv, hs):
                    P.act(t1[:, hs, :], bv, AF.Sqrt, bias=64e-5, scale=1.0 / 64)
                ones_mm(t1, ev_var)
                P.op('dve', lambda e: e.reciprocal(out=t1[:], in_=t1[:]), [t1[:]], [t1[:]])
                P.tt('dve', yf[:], yf[:], t1[:], ALU.mult)
                P.tt('pool', yf[:], yf[:], bc_t(VEC[:, 7, :]), ALU.mult)
                P.tt('pool', yf[:], yf[:], bc_t(VEC[:, 8, :]), ALU.add)
                P.tt('dve', kf[:], kf[:], kb[:, :, ::-1], ALU.add)
                P.tt('pool', kf[:], kf[:], rr[:], ALU.mult)
                P.tt('pool', t2[:], kf[:], bc_t(VEC[:, 6, :]), ALU.mult)
                ones_mm(t2, lambda bv, hs: P.stt(t2[:, hs, :], bv, 0.5, vT[:, hs, :], ALU.mult, ALU.mult))
                P.tt('dve', yf[:], yf[:], t2[:], ALU.add)
                P.tt('dve', Z[:], yf[:], gg[:], ALU.mult)
                w = 1 if t0 < LC else 0
                for dc in range(KC):
                    po = self.bank()
                    for h in range(16):
                        P.mm(po[:, :n], WO[:, h, dc * 128:(dc + 1) * 128], Z[:, h, :], start=(h == 0), stop=(h == 15))
                    gt = MOD[:, l, 2 * 8 + dc, w:w + 1]
                    P.stt(XA[:, dc, t0:t0 + n], po[:, :n], gt, XA[:, dc, t0:t0 + n], ALU.mult, ALU.add)

    def ffn(self, l, experts, tiles, moe):
        P = self.P
        XA, HB, MOD = self.XA, self.HB, self.MOD
        with self.scope():
            W1 = [self.sb("W1", [128, KC, 256], BF16) for _ in range(2)]
            W3 = [self.sb("W3", [128, KC, 256], BF16) for _ in range(2)]
            W2 = [self.sb("W2", [128, 2, D], BF16) for _ in range(2)]
            SI = [self.sb("SI", [128, 512]) for _ in range(2)]
            AF32 = [self.sb("AF32", [128, 512]) for _ in range(2)]
            ACT = [self.sb("ACT", [128, 2, 512], BF16) for _ in range(2)]
            GBE = self.sb("GBE", [128, T]) if moe else None
            g = 0
            it = 0
            for e, (w1, w3, w2) in enumerate(experts):
                if moe:
                    P.dma('sp', GBE[:], self.gb_scr[e])
                for fg in range(FC // 2):
                    a1, a3, a2 = W1[g % 2], W3[g % 2], W2[g % 2]
                    g += 1
                    P.dma('pool', a1[:], w1[:, fg * 256:(fg + 1) * 256].rearrange("(k p) m -> p k m", p=128))
                    P.dma('pool', a3[:], w3[:, fg * 256:(fg + 1) * 256].rearrange("(k p) m -> p k m", p=128))
                    P.dma('pool', a2[:], w2[fg * 256:(fg + 1) * 256, :].rearrange("(c p) m -> p c m", p=128))
                    for (t0, n, w) in tiles:
                        actt = ACT[it % 2]
                        for fc in range(2):
                            o1, o3 = self.bank(), self.bank()
                            for k in range(KC):
                                P.mm(o1[:, :n], a1[:, k, fc * 128:(fc + 1) * 128], HB[:, k, t0:t0 + n], start=(k == 0), stop=(k == KC - 1))
                            for k in range(KC):
                                P.mm(o3[:, :n], a3[:, k, fc * 128:(fc + 1) * 128], HB[:, k, t0:t0 + n], start=(k == 0), stop=(k == KC - 1))
                            si = SI[fc]
                            P.act(si[:, :n], o1[:, :n], AF.Silu)
                            if moe:
                                af = AF32[fc]
                                P.tt('dve', af[:, :n], si[:, :n], o3[:, :n], ALU.mult)
                                P.tt('pool', actt[:, fc, :n], af[:, :n], GBE[:, t0:t0 + n], ALU.mult)
                            else:
                                P.tt('dve', actt[:, fc, :n], si[:, :n], o3[:, :n], ALU.mult)
                        it += 1
                        for dc in range(KC):
                            y = self.bank()
                            for fc in range(2):
                                P.mm(y[:, :n], a2[:, fc, dc * 128:(dc + 1) * 128], actt[:, fc, :n], start=(fc == 0), stop=(fc == 1))
                            gt = MOD[:, l, 5 * 8 + dc, w:w + 1]
                            P.stt(XA[:, dc, t0:t0 + n], y[:, :n], gt, XA[:, dc, t0:t0 + n], ALU.mult, ALU.add)

    def final_norm(self, GFIN):
        P = self.P
        XA = self.XA
        with self.scope():
            sq = self.sb("fsq", [128, KC, 256])
            rs = self.sb("frs", [128, 256])
            OT = [self.sb("OT", [128, 256]) for _ in range(3)]
            i = 0
            for (t0, n, w) in TILES256[1:]:
                self.rstd_tile(t0, n, sq, rs)
                for c in range(KC):
                    ot = OT[i % 3]
                    i += 1
                    P.stt(ot[:], XA[:, c, t0:t0 + n], GFIN[:, c:c + 1], rs[:], ALU.mult, ALU.mult)
                    P.dma('sp' if i % 2 == 0 else 'act', self.outT[c * 128:(c + 1) * 128, t0 - LC:t0 - LC + n], ot[:])


def _fm(v):
    v = np.asarray(v, np.float32)
    lead = v.shape[:-1]
    r = v.reshape(lead + (KC, 128))
    r = np.moveaxis(r, -1, 0)
    return np.ascontiguousarray(r)


def _fm_n(v, nch):
    v = np.asarray(v, np.float32)
    lead = v.shape[:-1]
    r = v.reshape(lead + (nch, 128))
    r = np.moveaxis(r, -1, 0)
    return np.ascontiguousarray(r)


def prepare_shared(I):
    S = {}
    f32 = np.float32
    S["ada_w"] = np.ascontiguousarray(I["ada_w"], f32)
    S["ada_b"] = _fm_n(I["ada_b"], 48)
    S["gmix"] = _fm(I["norm_mix_g"])
    S["gffn"] = _fm(I["norm_ffn_g"])
    S["gfin"] = _fm(I["final_g"])
    par = np.zeros((2, 2, 128, 3, 32), f32)
    lre = np.asarray(I["s5_lambda_re"], f32).reshape(2, 2, 32, 128)
    lim = np.asarray(I["s5_lambda_im"], f32).reshape(2, 2, 32, 128)
    ldt = np.repeat(np.asarray(I["s5_log_dt"], f32).reshape(2, 2, 32, 2, 1), 64, axis=4).reshape(2, 2, 32, 128)
    par[:, :, :, 0, :] = lre.transpose(0, 1, 3, 2)
    par[:, :, :, 1, :] = lim.transpose(0, 1, 3, 2)
    par[:, :, :, 2, :] = ldt.transpose(0, 1, 3, 2)
    S["s5_par"] = par
    Bp = np.zeros((2, 2, 32, 128, 2, 128), f32)
    Cp = np.zeros((2, 2, 32, 128, 2, 128), f32)
    for z, (nb, ncn) in enumerate((("s5_b_re", "s5_c_re"), ("s5_b_im", "s5_c_im"))):
        Bz = np.asarray(I[nb], f32)
        Cz = np.asarray(I[ncn], f32)
        for g in range(64):
            sc, g2 = g // 2, g % 2
            chb = (g % 8) * 16
            Bp[:, :, sc, chb:chb + 16, z, g2 * 64:(g2 + 1) * 64] = Bz[:, :, g].transpose(0, 1, 3, 2)
            Cp[:, :, sc, g2 * 64:(g2 + 1) * 64, z, chb:chb + 16] = Cz[:, :, g].transpose(0, 1, 3, 2)
    S["s5_B"] = Bp
    S["s5_C"] = Cp
    S["s5_d"] = _fm(I["s5_d"])
    S["s5_glu_w"] = np.ascontiguousarray(I["s5_glu_w"], f32)
    S["s5_glu_b"] = _fm_n(I["s5_glu_b"], 16)
    S["cv_pw1_w"] = np.ascontiguousarray(I["cv_pw1_w"], f32)
    S["cv_pw1_b"] = _fm_n(I["cv_pw1_b"][0], 16)
    S["cv_dw_w"] = np.ascontiguousarray(np.asarray(I["cv_dw_w"], f32)[0].reshape(31, KC, 128).transpose(2, 1, 0))
    S["cv_vec"] = _fm(np.stack([I["cv_dw_b"][0], I["cv_ln_g"][0], I["cv_ln_b"][0], I["cv_pw2_b"][0]]))
    S["cv_pw2_w"] = np.ascontiguousarray(I["cv_pw2_w"], f32)
    for k in ("ff_w1", "ff_w3", "ff_w2", "moe_w1", "moe_w3", "moe_w2"):
        S[k] = np.ascontiguousarray(I[k], f32)
    S["rw_mu"] = _fm(I["rw_mu"][0])
    for k in ("r", "k", "v"):
        S["rw_w_" + k] = np.ascontiguousarray(I["rw_w_" + k][0], f32)
    S["rw_w_o"] = np.ascontiguousarray(np.asarray(I["rw_w_o"][0], f32).reshape(16, 64, D).transpose(1, 0, 2))
    for k in ("rw_w1", "rw_w2", "rw_a1", "rw_a2", "rw_g1", "rw_g2"):
        S[k] = np.ascontiguousarray(I[k][0], f32)
    def hv(v):
        return np.asarray(v, f32).reshape(16, 64).T
    S["rw_vec"] = np.ascontiguousarray(np.stack([hv(I["rw_w0"][0, 0]), hv(I["rw_w0"][0, 1]), hv(I["rw_a0"][0, 0]), hv(I["rw_a0"][0, 1]),
                                                 hv(I["rw_k_k"][0]), hv(I["rw_k_a"][0]), hv(I["rw_r_k"][0]),
                                                 hv(I["rw_lnx_g"][0]), hv(I["rw_lnx_b"][0])], axis=1))
    S["moe_router"] = np.ascontiguousarray(np.asarray(I["moe_router"], f32).reshape(2, KC, 128, NE).transpose(0, 2, 1, 3))
    return S


def prepare_core(I, b):
    f32 = np.float32
    xcat = np.concatenate([np.asarray(I["ctx"][b], f32), np.asarray(I["x"][b], f32)], axis=0)
    m = {"xT": np.ascontiguousarray(xcat.T)}
    cv = np.stack([np.asarray(I["c"][b], f32), np.asarray(I["c_ctx"], f32)], axis=-1)
    m["cvec"] = np.ascontiguousarray(cv.reshape(KC, 128, 2).transpose(1, 0, 2))
    return m


_CACHE = {}


NB = 1
CORES_PER_LAUNCH = 2


def run(inputs, n_layers=4, dbg=None, cores=None, nb=NB, n_batch=8):
    if cores is None:
        cores = CORES_PER_LAUNCH
    key = (n_layers, dbg, nb)
    if key not in _CACHE:
        bld = Builder(n_layers, dbg, nb)
        nc = bld.build()
        _CACHE[key] = (bld, nc)
    bld, nc = _CACHE[key]
    S = prepare_shared(inputs)
    outs = []
    per_launch = cores * nb
    for b0 in range(0, n_batch, per_launch):
        in_maps = []
        for c in range(cores):
            m = dict(S)
            pcs = [prepare_core(inputs, b0 + c * nb + i) for i in range(nb)]
            for k in pcs[0]:
                m[k] = np.ascontiguousarray(np.stack([p[k] for p in pcs], axis=0))
            in_maps.append(m)
        res = run_bass_kernel_spmd(nc, in_maps, core_ids=list(range(cores)))
        for c in range(cores):
            o = res.results[c]["outT"]
            for i in range(nb):
                outs.append(np.ascontiguousarray(o[i].T))
    return np.stack(outs, axis=0).astype(np.float32)


def kernel(**inputs):
    return run(inputs)
```
